# Optimizing a Trainium2 kernel written in Bass

```python
import jax
import jax.numpy as jnp
from jax import lax
import numpy as np

D_MODEL = 1024
BATCH = 4
SEQ = 8192
DEPTH = 2

ATT_HEADS = 8
ATT_HEAD_DIM = 64
ATT_WIDTH = ATT_HEADS * ATT_HEAD_DIM
IDX_HEADS = 4
IDX_DIM = 64
TOPK_MAX = 256
Q_BLOCK = 128
M_HEADS = 4
M_HEAD_DIM = 128
M_WIDTH = M_HEADS * M_HEAD_DIM
CHUNK = 128
CONV_K = 4
D_FF = 2816
ROPE_THETA = 10000.0
EPS = 1e-6
IN_SIZES = (ATT_WIDTH, ATT_HEAD_DIM, ATT_HEAD_DIM, IDX_HEADS * IDX_DIM, IDX_DIM, IDX_HEADS, 2 * M_WIDTH, M_WIDTH, M_WIDTH, M_HEADS, M_HEADS)
IN_COLS = ATT_WIDTH + 2 * ATT_HEAD_DIM + IDX_HEADS * IDX_DIM + IDX_DIM + IDX_HEADS + 4 * M_WIDTH + 2 * M_HEADS

kernel_name = "hybrid_dsa_mlstm_macaron"


def rmsnorm(x, g):
    xf = x.astype(jnp.float32)
    y = xf * lax.rsqrt(jnp.mean(xf * xf, axis=-1, keepdims=True) + EPS) * g.astype(jnp.float32)
    return y.astype(x.dtype)


def swiglu(x, w_gate, w_up, w_down):
    return (jax.nn.silu(x @ w_gate) * (x @ w_up)) @ w_down


def rope(x, pos):
    half = x.shape[-1] // 2
    inv_freq = ROPE_THETA ** (-jnp.arange(half, dtype=jnp.float32) / half)
    ang = pos.astype(jnp.float32)[:, None] * inv_freq[None, :]
    cos = jnp.cos(ang)[:, None, :]
    sin = jnp.sin(ang)[:, None, :]
    xf = x.astype(jnp.float32)
    x1, x2 = xf[..., :half], xf[..., half:]
    return jnp.concatenate([x1 * cos - x2 * sin, x2 * cos + x1 * sin], axis=-1).astype(x.dtype)


def causal_conv(x, w, b):
    y = lax.conv_general_dilated(x, w.astype(x.dtype), window_strides=(1,), padding=[(CONV_K - 1, 0)],
                                 dimension_numbers=("NWC", "WIO", "NWC"), feature_group_count=x.shape[-1])
    return y + b


def split_columns(proj):
    offsets = np.cumsum(IN_SIZES)[:-1].tolist()
    return jnp.split(proj, offsets, axis=-1)


def dsa_attention(q, k, v, q_idx, k_idx, w_idx):
    B, S = q.shape[0], q.shape[1]
    topk = min(TOPK_MAX, S // 4)
    nb = S // Q_BLOCK
    key_pos = jnp.arange(S, dtype=jnp.int32)

    def to_blocks(a):
        return jnp.moveaxis(a.reshape((B, nb, Q_BLOCK) + a.shape[2:]), 1, 0)

    def gather(src, idx):
        return jax.vmap(lambda sb, ib: sb[ib])(src, idx)

    def one_block(args):
        qb, qib, wb, start = args
        q_pos = start + jnp.arange(Q_BLOCK, dtype=jnp.int32)
        idx_logits = jnp.einsum("bqhd,bsd->bqhs", qib, k_idx, preferred_element_type=jnp.float32) * IDX_DIM ** -0.5
        index_score = jnp.einsum("bqhs,bqh->bqs", jax.nn.relu(idx_logits), wb.astype(jnp.float32))
        index_score = jnp.where(key_pos[None, None, :] <= q_pos[None, :, None], index_score, -jnp.inf)
        _, sel = lax.top_k(index_score, topk)
        k_sel = gather(k, sel)
        v_sel = gather(v, sel)
        logits = jnp.einsum("bqhd,bqkd->bqhk", qb, k_sel, preferred_element_type=jnp.float32) * ATT_HEAD_DIM ** -0.5
        valid = (sel <= q_pos[None, :, None])[:, :, None, :]
        p = jax.nn.softmax(jnp.where(valid, logits, -jnp.inf), axis=-1)
        return jnp.einsum("bqhk,bqkd->bqhd", p.astype(v_sel.dtype), v_sel)

    starts = jnp.arange(nb, dtype=jnp.int32) * Q_BLOCK
    out = lax.map(one_block, (to_blocks(q), to_blocks(q_idx), to_blocks(w_idx), starts))
    return jnp.moveaxis(out, 0, 1).reshape(B, S, ATT_WIDTH)


def mlstm(q, k, v, i_pre, f_pre):
    B, S, NH, d = q.shape
    nc = S // CHUNK

    def chunks(a):
        a = a.astype(jnp.float32).reshape((B, nc, CHUNK, NH) + a.shape[3:])
        return jnp.moveaxis(a, 3, 1)

    qc = chunks(q)
    kc = chunks(k) * d ** -0.5
    vc = chunks(v)
    ic = chunks(i_pre)
    b = jnp.cumsum(jax.nn.log_sigmoid(chunks(f_pre)), axis=-1)
    b_last = b[..., -1]

    a = b_last[..., None] - b + ic
    a_max = a.max(axis=-1)
    wa = jnp.exp(a - a_max[..., None])
    c_chunk = jnp.einsum("bhcl,bhcld,bhcle->bhcde", wa, kc, vc)
    n_chunk = jnp.einsum("bhcl,bhcld->bhcd", wa, kc)

    def step(carry, xs):
        C, n, m = carry
        bl, am, Cc, ncu = xs
        m_new = jnp.maximum(bl + m, am)
        s_old = jnp.exp(bl + m - m_new)
        s_new = jnp.exp(am - m_new)
        C_new = s_old[..., None, None] * C + s_new[..., None, None] * Cc
        n_new = s_old[..., None] * n + s_new[..., None] * ncu
        return (C_new, n_new, m_new), (C, n, m)

    init = (jnp.zeros((B, NH, d, d), jnp.float32), jnp.zeros((B, NH, d), jnp.float32), jnp.zeros((B, NH), jnp.float32))
    xs = (jnp.moveaxis(b_last, 2, 0), jnp.moveaxis(a_max, 2, 0), jnp.moveaxis(c_chunk, 2, 0), jnp.moveaxis(n_chunk, 2, 0))
    _, (C_prev, n_prev, m_prev) = lax.scan(step, init, xs)
    C_prev = jnp.moveaxis(C_prev, 0, 2)
    n_prev = jnp.moveaxis(n_prev, 0, 2)
    m_prev = jnp.moveaxis(m_prev, 0, 2)

    g = b + m_prev[..., None]
    causal = jnp.tril(jnp.ones((CHUNK, CHUNK), dtype=bool))
    dmat = jnp.where(causal, b[..., :, None] - b[..., None, :] + ic[..., None, :], -jnp.inf)
    m_t = jnp.maximum(g, dmat.max(axis=-1))
    w_intra = jnp.exp(dmat - m_t[..., None])
    w_inter = jnp.exp(g - m_t)
    s = jnp.einsum("bhcld,bhcsd->bhcls", qc, kc) * w_intra
    num = w_inter[..., None] * jnp.einsum("bhcld,bhcde->bhcle", qc, C_prev) + jnp.einsum("bhcls,bhcse->bhcle", s, vc)
    den = w_inter * jnp.einsum("bhcld,bhcd->bhcl", qc, n_prev) + s.sum(axis=-1)
    h = num / jnp.maximum(jnp.abs(den), jnp.exp(-m_t))[..., None]
    return jnp.moveaxis(h, 1, 3).reshape(B, S, NH, d)


def hybrid_mixer(h, pos, w_in, conv_w, conv_b, gate_b, q_norm, k_norm, m_norm, w_out):
    B, S, _ = h.shape
    q, k, v, q_idx, k_idx, w_idx, qk_m, v_m, o_m, i_pre, f_pre = split_columns(h @ w_in)
    q = rope(rmsnorm(q.reshape(B, S, ATT_HEADS, ATT_HEAD_DIM), q_norm), pos)
    k = rope(rmsnorm(k, k_norm)[:, :, None, :], pos)[:, :, 0, :]
    q_idx = rope(q_idx.reshape(B, S, IDX_HEADS, IDX_DIM), pos)
    k_idx = rope(k_idx[:, :, None, :], pos)[:, :, 0, :]
    att = dsa_attention(q, k, v, q_idx, k_idx, w_idx * IDX_HEADS ** -0.5)
    qk_m = jax.nn.silu(causal_conv(qk_m, conv_w, conv_b))
    q_m, k_m = jnp.split(qk_m, 2, axis=-1)
    shape_m = (B, S, M_HEADS, M_HEAD_DIM)
    h_m = mlstm(q_m.reshape(shape_m), k_m.reshape(shape_m), v_m.reshape(shape_m),
                i_pre + gate_b[:M_HEADS], f_pre + gate_b[M_HEADS:])
    h_m = rmsnorm(h_m, m_norm).reshape(B, S, M_WIDTH).astype(h.dtype) * jax.nn.sigmoid(o_m)
    return jnp.concatenate([att, h_m], axis=-1) @ w_out


def setup_inputs(seed: int = 0) -> dict:
    key = jax.random.key(seed)
    keys = jax.random.split(key, 19)

    def nrm(k, shape, scale):
        return scale * jax.random.normal(k, shape, jnp.float32)

    mix_w = ATT_WIDTH + M_WIDTH
    return {
        "x": nrm(keys[0], (BATCH, SEQ, D_MODEL), 1.0),
        "ffn1_norm": 1.0 + nrm(keys[1], (DEPTH, D_MODEL), 0.02),
        "ffn1_w_gate": nrm(keys[2], (DEPTH, D_MODEL, D_FF), D_MODEL ** -0.5),
        "ffn1_w_up": nrm(keys[3], (DEPTH, D_MODEL, D_FF), D_MODEL ** -0.5),
        "ffn1_w_down": nrm(keys[4], (DEPTH, D_FF, D_MODEL), D_FF ** -0.5),
        "mix_norm": 1.0 + nrm(keys[5], (DEPTH, D_MODEL), 0.02),
        "w_in": nrm(keys[6], (DEPTH, D_MODEL, IN_COLS), D_MODEL ** -0.5),
        "conv_w": nrm(keys[7], (DEPTH, CONV_K, 1, 2 * M_WIDTH), CONV_K ** -0.5),
        "conv_b": nrm(keys[8], (DEPTH, 2 * M_WIDTH), 0.02),
        "gate_b": jnp.concatenate([nrm(keys[9], (DEPTH, M_HEADS), 0.1),
                                   3.0 + nrm(keys[10], (DEPTH, M_HEADS), 0.5)], axis=-1),
        "q_norm": 1.0 + nrm(keys[11], (DEPTH, ATT_HEAD_DIM), 0.02),
        "k_norm": 1.0 + nrm(keys[12], (DEPTH, ATT_HEAD_DIM), 0.02),
        "m_norm": 1.0 + nrm(keys[13], (DEPTH, M_HEADS, M_HEAD_DIM), 0.02),
        "w_out": nrm(keys[14], (DEPTH, mix_w, D_MODEL), mix_w ** -0.5),
        "ffn2_norm": 1.0 + nrm(keys[15], (DEPTH, D_MODEL), 0.02),
        "ffn2_w_gate": nrm(keys[16], (DEPTH, D_MODEL, D_FF), D_MODEL ** -0.5),
        "ffn2_w_up": nrm(keys[17], (DEPTH, D_MODEL, D_FF), D_MODEL ** -0.5),
        "ffn2_w_down": nrm(keys[18], (DEPTH, D_FF, D_MODEL), D_FF ** -0.5),
    }


def reference(x, ffn1_norm, ffn1_w_gate, ffn1_w_up, ffn1_w_down, mix_norm, w_in, conv_w, conv_b, gate_b,
              q_norm, k_norm, m_norm, w_out, ffn2_norm, ffn2_w_gate, ffn2_w_up, ffn2_w_down):
    pos = jnp.arange(x.shape[1], dtype=jnp.int32)
    for l in range(DEPTH):
        x = x + 0.5 * swiglu(rmsnorm(x, ffn1_norm[l]), ffn1_w_gate[l], ffn1_w_up[l], ffn1_w_down[l])
        x = x + hybrid_mixer(rmsnorm(x, mix_norm[l]), pos, w_in[l], conv_w[l], conv_b[l], gate_b[l],
                             q_norm[l], k_norm[l], m_norm[l], w_out[l])
        x = x + 0.5 * swiglu(rmsnorm(x, ffn2_norm[l]), ffn2_w_gate[l], ffn2_w_up[l], ffn2_w_down[l])
    return x
```

```python
from contextlib import ExitStack
import numpy as np
import concourse.bass as bass
import concourse.mybir as mybir
from concourse.bass_utils import run_bass_kernel_spmd

F32 = mybir.dt.float32
BF16 = mybir.dt.bfloat16
ALU = mybir.AluOpType
AF = mybir.ActivationFunctionType
AX = mybir.AxisListType

SEM_CH = 30000


class Buf:
    __slots__ = ("name", "writes", "reads", "dsem", "dtotal")

    def __init__(self, name):
        self.name = name
        self.writes = []
        self.reads = []
        self.dsem = None
        self.dtotal = 0


class Inst:
    __slots__ = ("fn", "waits", "mark", "rank", "dma", "clock")

    def __init__(self, fn):
        self.fn = fn
        self.waits = []
        self.mark = False
        self.rank = 0
        self.dma = None
        self.clock = None


class Sched:
    ENG = {"pe": "tensor", "act": "scalar", "dve": "vector", "pool": "gpsimd", "sp": "sync"}

    def __init__(self, nc):
        self.nc = nc
        self.insts = {k: [] for k in self.ENG}
        self.waited = {k: {} for k in self.ENG}
        self.ndsem = 0
        self.dma_clock = {}
        self.es = ExitStack()
        self.nbuf = 0

    def sb(self, name, shape, dt):
        return self.es.enter_context(self.nc.sbuf_tensor("sb_" + name, list(shape), dt))

    def ps(self, name, shape, dt):
        return self.es.enter_context(self.nc.psum_tensor("ps_" + name, list(shape), dt))

    def buf(self, name=None):
        self.nbuf += 1
        return Buf(name or f"b{self.nbuf}")

    def _resolve(self, eng, deps):
        W = self.waited[eng]
        best = {}
        for ev in deps:
            key = ev[1]
            if ev[0] == "e":
                if ev[1] == "pe" and eng == "pe":
                    continue
                k = ("e", key)
            else:
                k = ("d", key)
            if best.get(k, 0) < ev[2]:
                best[k] = ev[2]
        final = []
        for (kind, key), val in best.items():
            wk = key if kind == "e" else ("d", key)
            if W.get(wk, 0) >= val:
                continue
            W[wk] = val
            final.append((kind, key, val))
            if kind == "e":
                src = self.insts[key][val - 1]
                src.mark = True
                clk = src.clock
            else:
                clk = self.dma_clock[(key, val)]
            for a, b in clk.items():
                if W.get(a, 0) < b:
                    W[a] = b
        return final

    def op(self, eng, fn, reads=(), writes=()):
        deps = set()
        for b in reads:
            deps.update(b.writes)
        for b in writes:
            deps.update(b.writes)
            deps.update(b.reads)
        inst = Inst(fn)
        inst.waits = self._resolve(eng, deps)
        self.insts[eng].append(inst)
        idx = len(self.insts[eng])
        inst.clock = dict(self.waited[eng])
        ev = ("e", eng, idx)
        for b in writes:
            b.writes = [ev]
            b.reads = []
        for b in reads:
            if b not in writes:
                b.reads.append(ev)
        return ev

    def dma(self, q, out, in_, reads=(), writes=(), owner=None, **kw):
        deps = set()
        for b in reads:
            deps.update(b.writes)
        for b in writes:
            deps.update(b.writes)
            deps.update(b.reads)
        if owner is None:
            owner = writes[0]
        if owner.dsem is None:
            owner.dsem = self.ndsem
            self.ndsem += 1
        inst = Inst(lambda e: e.dma_start(out=out, in_=in_, **kw))
        inst.waits = self._resolve(q, deps)
        owner.dtotal += 16
        inst.dma = (owner.dsem, 16)
        self.insts[q].append(inst)
        inst.clock = dict(self.waited[q])
        ev = ("d", owner.dsem, owner.dtotal)
        self.dma_clock[(owner.dsem, owner.dtotal)] = inst.clock
        for b in writes:
            b.writes = [ev]
            b.reads = []
        for b in reads:
            if b not in writes:
                b.reads.append(ev)
        return ev

    def fence(self, eng, bufs):
        deps = set()
        for b in bufs:
            deps.update(b.writes)
            deps.update(b.reads)
        inst = Inst(None)
        inst.waits = self._resolve(eng, deps)
        self.insts[eng].append(inst)
        inst.clock = dict(self.waited[eng])

    def emit(self):
        nc = self.nc
        es = self.es
        esem = {}
        for eng, lst in self.insts.items():
            r = 0
            for inst in lst:
                if inst.mark:
                    r += 1
                    inst.rank = r
            nsem = (r + SEM_CH - 1) // SEM_CH
            esem[eng] = [es.enter_context(nc.semaphore(f"s_{eng}{i}")) for i in range(nsem)]
        dsem = [es.enter_context(nc.semaphore(f"d{i}")) for i in range(self.ndsem)]
        self.nsem_total = sum(len(v) for v in esem.values()) + len(dsem)
        block = es.enter_context(nc.Block())
        insts = self.insts

        def make_body(eng):
            def body(eh):
                for inst in insts[eng]:
                    for (kind, key, val) in inst.waits:
                        if kind == "e":
                            rk = insts[key][val - 1].rank
                            assert rk > 0
                            eh.wait_ge(esem[key][(rk - 1) // SEM_CH], (rk - 1) % SEM_CH + 1)
                        else:
                            eh.wait_ge(dsem[key], val)
                    if inst.fn is None:
                        continue
                    r = inst.fn(eh)
                    if inst.dma is not None:
                        r.then_inc(dsem[inst.dma[0]], inst.dma[1])
                    elif inst.mark:
                        r.then_inc(esem[eng][(inst.rank - 1) // SEM_CH], 1)
            return body

        for eng, attr in self.ENG.items():
            if insts[eng]:
                getattr(block, attr)(make_body(eng))
        es.close()


OFF = dict(q_att=0, k_att=512, v_att=576, q_idx=640, k_idx=896, w_idx=960, qk_m=964, v_m=1988, o_m=2500, i=3012, f=3016)


def make_consts():
    c = np.zeros((128, 384), np.float32)
    c[:, 0:128] = 1.0
    c[0:64, 128:192] = 1.0
    c[64:128, 192:256] = 1.0
    rt = np.zeros((128, 128), np.float32)
    for m in range(128):
        if m % 64 < 32:
            rt[m + 32, m] = -1.0
        else:
            rt[m - 32, m] = 1.0
    c[:, 256:384] = rt
    return c


def prep_wgu(wg, wu):
    a = np.stack([wg, wu], 0).reshape(2, 8, 128, 22, 128)
    return np.ascontiguousarray(a.transpose(3, 2, 0, 1, 4)).reshape(22, 128, 2 * 8 * 128)


def prep_wd(wd):
    a = wd.reshape(22, 128, 8, 128)
    return np.ascontiguousarray(a.transpose(2, 1, 0, 3)).reshape(8, 128, 22 * 128)


def prep_g(g):
    return np.ascontiguousarray(g.reshape(8, 128).T)


def colF():
    cols = []
    for c in range(4):
        cols.append(np.arange(c * 128, (c + 1) * 128))
    for c in range(2):
        cols.append(OFF["q_idx"] + np.arange(c * 128, (c + 1) * 128))
    k = OFF["k_att"] + np.arange(64)
    cols.append(np.concatenate([k, k]))
    k = OFF["k_idx"] + np.arange(64)
    cols.append(np.concatenate([k, k]))
    for c in range(8):
        cols.append(OFF["qk_m"] + np.arange(c * 128, (c + 1) * 128))
    return np.stack(cols, 0)


def colT():
    return np.concatenate([OFF["v_att"] + np.arange(64), OFF["w_idx"] + np.arange(4), OFF["i"] + np.arange(4), OFF["f"] + np.arange(4),
                           OFF["v_m"] + np.arange(512), OFF["o_m"] + np.arange(512)])


def prep_winF(w_in):
    cf = colF()
    a = w_in[:, cf.reshape(-1)].reshape(8, 128, 16, 128)
    return np.ascontiguousarray(a.transpose(1, 2, 0, 3)).reshape(128, 16 * 8 * 128)


def prep_winT(w_in):
    a = w_in[:, colT()].reshape(8, 128, 1100)
    return np.ascontiguousarray(a.transpose(1, 0, 2)).reshape(128, 8 * 1100)


def prep_wout(w_out):
    a = w_out.reshape(8, 128, 1024)
    return np.ascontiguousarray(a.transpose(1, 0, 2)).reshape(128, 8 * 1024)


def prep_gqk(qn, kn):
    return np.ascontiguousarray(np.stack([np.concatenate([qn, qn]), np.concatenate([kn, kn])], 1)).astype(np.float32)


def rope_tables(pos):
    half = 32
    inv = (np.float32(10000.0) ** (-np.arange(half, dtype=np.float32) / np.float32(half))).astype(np.float32)
    ang = pos.astype(np.float32)[None, :] * inv[:, None]
    c = np.cos(ang).astype(np.float32)
    s = np.sin(ang).astype(np.float32)
    return np.ascontiguousarray(np.tile(c, (4, 1))), np.ascontiguousarray(np.tile(s, (4, 1)))


D = 1024
DFF = 2816
NFC = 22
KC = 8
NF = 16
NT = 1100
EPS = 1e-6
TGRP = [(0, 76), (76, 588), (588, 1100)]


def build_tok(has_post, has_pre, T=4096, TT=512):
    nc = bass.Bass("TRN2", target_bir_lowering=False)
    S = Sched(nc)
    NTILE = T // TT

    def din(name, shape, dt=F32):
        return nc.dram_tensor(name, list(shape), dt, kind="ExternalInput").ap()

    def dout(name, shape, dt=F32):
        return nc.dram_tensor(name, list(shape), dt, kind="ExternalOutput").ap()

    xin = din("xin", [D, T])
    consts = din("consts", [128, 384])
    if has_post:
        mix = din("mix", [D, T], BF16)
        wout = din("wout", [128, KC * D])
        wgu2 = din("wgu2", [NFC, 128, 2 * KC * 128])
        wd2 = din("wd2", [KC, 128, NFC * 128])
        gn2 = din("gn2", [128, KC])
    if has_pre:
        wgu1 = din("wgu1", [NFC, 128, 2 * KC * 128])
        wd1 = din("wd1", [KC, 128, NFC * 128])
        gn1 = din("gn1", [128, KC])
        gnm = din("gnm", [128, KC])
        winF = din("winF", [128, NF * KC * 128])
        winT = din("winT", [128, KC * NT])
        gqk = din("gqk", [128, 2])
        cosT = din("cosT", [128, T])
        sinT = din("sinT", [128, T])
        x1o = dout("x1o", [D, T])
        fbf = dout("fbf", [8, 128, T], BF16)
        qkm = dout("qkm", [8, 128, T])
        tokT = dout("tokT", [T, NT])
    else:
        xo = dout("xo", [D, T])

    xt = S.sb("xt", [128, KC, TT], F32); b_xt = [S.buf() for _ in range(KC)]
    sq = S.sb("sq", [128, KC, TT], F32); b_sq = S.buf()
    xn = S.sb("xn", [128, KC, TT], BF16); b_xn = [S.buf() for _ in range(KC)]
    rs = S.sb("rs", [128, TT], F32); b_rs = S.buf()
    rstd = S.sb("rstd", [128, TT], F32); b_rstd = S.buf()
    actt = S.sb("actt", [128, NFC, TT], BF16); b_act = [S.buf() for _ in range(NFC)]
    sg = [S.sb(f"sg{i}", [128, TT], F32) for i in range(2)]; b_sg = [S.buf() for _ in range(2)]
    NW = 3
    wgu = [S.sb(f"wgu{i}", [128, 2, KC, 128], BF16) for i in range(NW)]; b_wgu = [S.buf() for _ in range(NW)]
    wd = [S.sb(f"wd{i}", [128, NFC, 128], BF16) for i in range(2)]; b_wd = [S.buf() for _ in range(2)]
    cst = S.sb("cst", [128, 384], F32); b_cst = S.buf()
    rtb = S.sb("rtb", [128, 128], BF16); b_rtb = S.buf()
    gv = S.sb("gv", [128, 4 * KC + 2], F32); b_gv = S.buf()
    P = [S.ps(f"P{i}", [128, 512], F32) for i in range(8)]; b_P = [S.buf() for _ in range(8)]
    if has_post:
        mixt = S.sb("mixt", [128, KC, TT], BF16); b_mixt = S.buf()
        woutb = S.sb("woutb", [128, KC, D], BF16); b_wout = S.buf()
    if has_pre:
        winFb = S.sb("winFb", [128, NF, KC, 128], BF16); b_winF = S.buf()
        winTb = S.sb("winTb", [128, KC, NT], BF16); b_winT = S.buf()
        cs = S.sb("cs", [128, 2, TT], F32); b_cs = S.buf()
        qn = [S.sb(f"qn{i}", [128, TT], BF16) for i in range(2)]; b_qn = [S.buf() for _ in range(2)]
        t1 = [S.sb(f"t1{i}", [128, TT], F32) for i in range(2)]; b_t1 = [S.buf() for _ in range(2)]
        t2 = [S.sb(f"t2{i}", [128, TT], F32) for i in range(2)]; b_t2 = [S.buf() for _ in range(2)]
        qo = [S.sb(f"qo{i}", [128, TT], BF16) for i in range(2)]; b_qo = [S.buf() for _ in range(2)]
        fo = [S.sb(f"fo{i}", [128, TT], F32) for i in range(2)]; b_fo = [S.buf() for _ in range(2)]
        tko = [S.sb(f"tko{i}", [128, NT], F32) for i in range(2)]; b_tko = [S.buf() for _ in range(2)]
    outbufs = []

    S.dma("sp", cst[:], consts, writes=[b_cst])
    S.op("dve", lambda e: e.tensor_copy(out=rtb[:], in_=cst[:, 256:384]), reads=[b_cst], writes=[b_rtb])
    gcol = {}
    col = 0
    for nm, have in (("gn2", has_post), ("gn1", has_pre), ("gnm", has_pre)):
        if have:
            src = {"gn2": gn2 if has_post else None, "gn1": gn1 if has_pre else None, "gnm": gnm if has_pre else None}[nm]
            S.dma("sp", gv[:, col:col + KC], src, writes=[b_gv])
            gcol[nm] = col
            col += KC
    if has_pre:
        S.dma("sp", gv[:, col:col + 2], gqk, writes=[b_gv])
        gcol["gqk"] = col
        S.dma("pool", winFb[:].rearrange("p a b c -> p (a b c)"), winF, writes=[b_winF])
        S.dma("pool", winTb[:].rearrange("p a b -> p (a b)"), winT, writes=[b_winT])
    if has_post:
        S.dma("pool", woutb[:].rearrange("p a b -> p (a b)"), wout, writes=[b_wout])
    ones = cst[:, 0:128]
    bones = cst[:, 128:256]

    state = {"w": 0, "d": 0, "g": 0}

    def rmsnorm(gname):
        g0 = gcol[gname]
        S.op("act", lambda e: e.activation(out=sq[:], in_=xt[:], func=AF.Square), reads=b_xt, writes=[b_sq])
        for kc in range(KC):
            S.op("pe", lambda e, kc=kc: e.matmul(P[6][:], lhsT=ones, rhs=sq[:, kc, :], start=(kc == 0), stop=(kc == KC - 1)),
                 reads=[b_sq, b_cst], writes=[b_P[6]])
        S.op("act", lambda e: e.activation(out=rs[:], in_=P[6][:], func=AF.Sqrt, bias=EPS, scale=1.0 / D), reads=[b_P[6]], writes=[b_rs])
        S.op("dve", lambda e: e.reciprocal(out=rstd[:], in_=rs[:]), reads=[b_rs], writes=[b_rstd])
        for kc in range(KC):
            S.op("dve", lambda e, kc=kc: e.scalar_tensor_tensor(out=xn[:, kc, :], in0=xt[:, kc, :], scalar=gv[:, g0 + kc:g0 + kc + 1],
                                                                in1=rstd[:], op0=ALU.mult, op1=ALU.mult),
                 reads=[b_xt[kc], b_rstd, b_gv], writes=[b_xn[kc]])

    def ffn(wgu_d, wd_d):
        for fc in range(NFC):
            s = state["w"] % NW; state["w"] += 1
            j = state["g"] % 2; state["g"] += 1
            S.dma("pool", wgu[s][:].rearrange("p a b c -> p (a b c)"), wgu_d[fc], writes=[b_wgu[s]])
            for gu in range(2):
                pb = P[0 + j] if gu == 0 else P[2 + j]
                bb = b_P[0 + j] if gu == 0 else b_P[2 + j]
                for kc in range(KC):
                    S.op("pe", lambda e, s=s, gu=gu, kc=kc, pb=pb: e.matmul(pb[:], lhsT=wgu[s][:, gu, kc, :], rhs=xn[:, kc, :],
                                                                             start=(kc == 0), stop=(kc == KC - 1)),
                         reads=[b_wgu[s], b_xn[kc]], writes=[bb])
            S.op("act", lambda e, j=j: e.activation(out=sg[j][:], in_=P[0 + j][:], func=AF.Silu), reads=[b_P[0 + j]], writes=[b_sg[j]])
            S.op("dve", lambda e, j=j, fc=fc: e.tensor_tensor(out=actt[:, fc, :], in0=P[2 + j][:], in1=sg[j][:], op=ALU.mult),
                 reads=[b_P[2 + j], b_sg[j]], writes=[b_act[fc]])
        for dc in range(KC):
            s = state["d"] % 2; state["d"] += 1
            S.dma("pool", wd[s][:].rearrange("p a b -> p (a b)"), wd_d[dc], writes=[b_wd[s]])
            for fc in range(NFC):
                S.op("pe", lambda e, s=s, fc=fc: e.matmul(P[4 + s][:], lhsT=wd[s][:, fc, :], rhs=actt[:, fc, :],
                                                           start=(fc == 0), stop=(fc == NFC - 1)),
                     reads=[b_wd[s], b_act[fc]], writes=[b_P[4 + s]])
            S.op("dve", lambda e, s=s, dc=dc: e.scalar_tensor_tensor(out=xt[:, dc, :], in0=P[4 + s][:], scalar=0.5, in1=xt[:, dc, :],
                                                                      op0=ALU.mult, op1=ALU.add),
                 reads=[b_P[4 + s], b_xt[dc]], writes=[b_xt[dc]])

    for ti in range(NTILE):
        tsl = slice(ti * TT, (ti + 1) * TT)
        S.dma("sp", xt[:], xin.rearrange("(kc p) t -> p kc t", p=128)[:, :, tsl], writes=b_xt)
        if has_post:
            S.dma("sp", mixt[:], mix.rearrange("(kc p) t -> p kc t", p=128)[:, :, tsl], writes=[b_mixt])
            for dc in range(KC):
                s = state["d"] % 2; state["d"] += 1
                for kc in range(KC):
                    S.op("pe", lambda e, s=s, kc=kc, dc=dc: e.matmul(P[4 + s][:], lhsT=woutb[:, kc, dc * 128:(dc + 1) * 128], rhs=mixt[:, kc, :],
                                                                      start=(kc == 0), stop=(kc == KC - 1)),
                         reads=[b_wout, b_mixt], writes=[b_P[4 + s]])
                S.op("dve", lambda e, s=s, dc=dc: e.tensor_tensor(out=xt[:, dc, :], in0=P[4 + s][:], in1=xt[:, dc, :], op=ALU.add),
                     reads=[b_P[4 + s], b_xt[dc]], writes=[b_xt[dc]])
            rmsnorm("gn2")
            ffn(wgu2, wd2)
        if not has_pre:
            ob = S.buf(); outbufs.append(ob)
            S.dma("sp", xo.rearrange("(kc p) t -> p kc t", p=128)[:, :, tsl], xt[:], reads=b_xt, writes=[ob], owner=b_xt[0])
            continue
        rmsnorm("gn1")
        ffn(wgu1, wd1)
        ob = S.buf(); outbufs.append(ob)
        S.dma("sp", x1o.rearrange("(kc p) t -> p kc t", p=128)[:, :, tsl], xt[:], reads=b_xt, writes=[ob], owner=b_xt[0])
        rmsnorm("gnm")
        S.dma("sp", cs[:, 0, :], cosT[:, tsl], writes=[b_cs])
        S.dma("sp", cs[:, 1, :], sinT[:, tsl], writes=[b_cs], owner=b_cs)
        for c in range(NF):
            j = c % 2
            for kc in range(KC):
                S.op("pe", lambda e, c=c, kc=kc, j=j: e.matmul(P[j][:], lhsT=winFb[:, c, kc, :], rhs=xn[:, kc, :], start=(kc == 0), stop=(kc == KC - 1)),
                     reads=[b_winF, b_xn[kc]], writes=[b_P[j]])
            if c >= 8:
                S.op("act", lambda e, j=j: e.activation(out=fo[j][:], in_=P[j][:], func=AF.Copy), reads=[b_P[j]], writes=[b_fo[j]])
                ob = S.buf(); outbufs.append(ob)
                S.dma("sp", qkm[c - 8][:, tsl], fo[j][:], reads=[b_fo[j]], writes=[ob], owner=b_fo[j])
                continue
            normed = (c < 4) or (c == 6)
            if normed:
                gc = gcol["gqk"] + (0 if c < 4 else 1)
                S.op("act", lambda e, j=j: e.activation(out=t1[j][:], in_=P[j][:], func=AF.Square), reads=[b_P[j]], writes=[b_t1[j]])
                S.op("pe", lambda e, j=j: e.matmul(P[6][:], lhsT=bones, rhs=t1[j][:], start=True, stop=True), reads=[b_t1[j], b_cst], writes=[b_P[6]])
                S.op("act", lambda e: e.activation(out=rs[:], in_=P[6][:], func=AF.Sqrt, bias=EPS, scale=1.0 / 64), reads=[b_P[6]], writes=[b_rs])
                S.op("dve", lambda e: e.reciprocal(out=rstd[:], in_=rs[:]), reads=[b_rs], writes=[b_rstd])
                S.op("dve", lambda e, j=j, gc=gc: e.scalar_tensor_tensor(out=qn[j][:], in0=P[j][:], scalar=gv[:, gc:gc + 1], in1=rstd[:],
                                                                          op0=ALU.mult, op1=ALU.mult),
                     reads=[b_P[j], b_rstd, b_gv], writes=[b_qn[j]])
            else:
                S.op("act", lambda e, j=j: e.activation(out=qn[j][:], in_=P[j][:], func=AF.Copy), reads=[b_P[j]], writes=[b_qn[j]])
            S.op("pe", lambda e, j=j: e.matmul(P[2 + j][:], lhsT=rtb[:], rhs=qn[j][:], start=True, stop=True), reads=[b_rtb, b_qn[j]], writes=[b_P[2 + j]])
            S.op("pool", lambda e, j=j: e.tensor_tensor(out=t1[j][:], in0=qn[j][:], in1=cs[:, 0, :], op=ALU.mult), reads=[b_qn[j], b_cs], writes=[b_t1[j]])
            S.op("dve", lambda e, j=j: e.tensor_tensor(out=t2[j][:], in0=P[2 + j][:], in1=cs[:, 1, :], op=ALU.mult), reads=[b_P[2 + j], b_cs], writes=[b_t2[j]])
            S.op("pool", lambda e, j=j: e.tensor_tensor(out=qo[j][:], in0=t1[j][:], in1=t2[j][:], op=ALU.add), reads=[b_t1[j], b_t2[j]], writes=[b_qo[j]])
            ob = S.buf(); outbufs.append(ob)
            S.dma("sp", fbf[c][:, tsl], qo[j][:], reads=[b_qo[j]], writes=[ob], owner=b_qo[j])
        for tb in range(TT // 128):
            j = tb % 2
            for gi, (c0, c1) in enumerate(TGRP):
                pb = 4 + (gi % 2)
                for kc in range(KC):
                    S.op("pe", lambda e, kc=kc, tb=tb, c0=c0, c1=c1, pb=pb: e.matmul(P[pb][:, 0:c1 - c0], lhsT=xn[:, kc, tb * 128:(tb + 1) * 128],
                                                                                      rhs=winTb[:, kc, c0:c1], start=(kc == 0), stop=(kc == KC - 1)),
                         reads=[b_winT, b_xn[kc]], writes=[b_P[pb]])
                eng = "act" if gi % 2 == 0 else "dve"
                if eng == "act":
                    S.op("act", lambda e, j=j, c0=c0, c1=c1, pb=pb: e.activation(out=tko[j][:, c0:c1], in_=P[pb][:, 0:c1 - c0], func=AF.Copy),
                         reads=[b_P[pb]], writes=[b_tko[j]])
                else:
                    S.op("dve", lambda e, j=j, c0=c0, c1=c1, pb=pb: e.tensor_copy(out=tko[j][:, c0:c1], in_=P[pb][:, 0:c1 - c0]),
                         reads=[b_P[pb]], writes=[b_tko[j]])
            ob = S.buf(); outbufs.append(ob)
            r0 = ti * TT + tb * 128
            S.dma("sp", tokT[r0:r0 + 128, :], tko[j][:], reads=[b_tko[j]], writes=[ob], owner=b_tko[j])
    S.fence("sp", outbufs)
    S.emit()
    return nc, S


TOPK = 256
NITER = 18
NEG = -1.0e30


def build_att(SEQ=8192, r_static=None):
    nc = bass.Bass("TRN2", target_bir_lowering=False)
    S = Sched(nc)
    NCH = SEQ // 128
    NQB = NCH // 2
    TQ = NQB * 128

    def din(name, shape, dt=F32):
        return nc.dram_tensor(name, list(shape), dt, kind="ExternalInput").ap()

    kT_d = din("kT", [128, SEQ], BF16)
    kiT_d = din("kiT", [128, SEQ], BF16)
    v_d = din("vtok", [SEQ, 64])
    qT_d = din("qT", [128, 4, TQ], BF16)
    qiT_d = din("qiT", [128, 2, TQ], BF16)
    w_d = din("widx", [128, NQB, 4])
    cm_d = din("cmask", [128, 256])
    consts = din("aconsts", [128, 256])
    att_d = nc.dram_tensor("attT", [64, 8, TQ], BF16, kind="ExternalOutput").ap()

    kT = S.sb("kT", [128, SEQ], BF16); b_kT = S.buf()
    kiT = S.sb("kiT", [128, SEQ], BF16); b_kiT = S.buf()
    v1 = S.sb("v1", [128, NCH, 65], BF16); b_v1 = S.buf()
    wq = S.sb("wq", [128, NQB, 4], F32); b_wq = S.buf()
    cm = S.sb("cm", [128, 256], F32); b_cm = S.buf()
    cst = S.sb("cst", [128, 256], F32); b_cst = S.buf()
    identb = S.sb("identb", [128, 128], BF16); b_identb = S.buf()
    qb_t = [S.sb(f"qbt{i}", [128, 4, 128], BF16) for i in range(2)]; b_qb = [S.buf() for _ in range(2)]
    qi_t = [S.sb(f"qit{i}", [128, 2, 128], BF16) for i in range(2)]; b_qi = [S.buf() for _ in range(2)]
    NKT = SEQ // 512
    Isc = S.sb("Isc", [128, SEQ], F32); b_Isc = [S.buf() for _ in range(NKT)]
    mask = S.sb("mask", [128, SEQ], BF16); b_mask = S.buf()
    maskT = S.sb("maskT", [128, NCH, 128], BF16); b_maskT = [S.buf() for _ in range((NCH + 7) // 8)]
    rt = [S.sb(f"rt{i}", [128, 512], F32) for i in range(2)]; b_rt = [S.buf() for _ in range(2)]
    E = [[S.sb(f"E{j}{eo}", [128, 512], BF16) for eo in range(2)] for j in range(2)]
    b_E = [[S.buf() for eo in range(2)] for j in range(2)]
    PT = [[S.sb(f"PT{j}{eo}", [128, 512], BF16) for eo in range(2)] for j in range(2)]
    b_PT = [[S.buf() for eo in range(2)] for j in range(2)]
    sm = S.sb("sm", [128, 16], F32)
    b_sm = {k: S.buf() for k in ("M", "W0", "lo", "cand", "cnt", "pw")}
    osb = S.sb("osb", [65, 1024], F32); b_osb = S.buf()
    rr = S.sb("rr", [65, 1024], F32); b_rr = S.buf()
    ao = [S.sb(f"ao{i}", [64, 1024], BF16) for i in range(2)]; b_ao = [S.buf() for _ in range(2)]
    P = [S.ps(f"P{i}", [128, 512], F32) for i in range(8)]; b_P = [S.buf() for _ in range(8)]
    PI = [0, 1]; PSe, PSo, POe, POo, MT0, MT1 = 2, 3, 4, 5, 6, 7
    outbufs = []

    S.dma("sp", cst[:], consts, writes=[b_cst])
    S.dma("sp", kT[:], kT_d, writes=[b_kT])
    S.dma("sp", kiT[:], kiT_d, writes=[b_kiT])
    S.dma("sp", wq[:], w_d, writes=[b_wq])
    S.dma("sp", cm[:], cm_d, writes=[b_cm])
    S.op("dve", lambda e: e.memset(v1[:], 1.0), writes=[b_v1])
    vv = v_d.rearrange("(c p) d -> p c d", p=128)
    for g in range(0, NCH, 8):
        S.dma("pool", v1[:, g:g + 8, 0:64], vv[:, g:g + 8, :], writes=[b_v1])
    S.op("dve", lambda e: e.tensor_copy(out=identb[:], in_=cst[:, 0:128]), reads=[b_cst], writes=[b_identb])
    ones_row = cst[64:65, 128:192]

    st = {"r": 0, "e": 0}

    def load_q(i):
        j = i % 2
        S.dma("sp", qb_t[j][:], qT_d[:, :, i * 128:(i + 1) * 128], writes=[b_qb[j]])
        S.dma("sp", qi_t[j][:], qiT_d[:, :, i * 128:(i + 1) * 128], writes=[b_qi[j]])

    def phaseA(i):
        j = i % 2
        nch = 2 * i + 2
        nk = nch * 128
        ntile = (nk + 511) // 512
        for kt in range(ntile):
            k0 = kt * 512
            kn = min(512, nk - k0)
            for h in range(4):
                half, c = h % 2, h // 2
                pj = st["r"] % 2; st["r"] += 1
                S.op("pe", lambda e, pj=pj, half=half, c=c, k0=k0, kn=kn, j=j: e.matmul(
                    P[PI[pj]][:, 0:kn], lhsT=qi_t[j][half * 64:(half + 1) * 64, c, :], rhs=kiT[half * 64:(half + 1) * 64, k0:k0 + kn],
                    start=True, stop=True), reads=[b_qi[j], b_kiT], writes=[b_P[PI[pj]]])
                S.op("act", lambda e, pj=pj, kn=kn: e.activation(out=rt[pj][:, 0:kn], in_=P[PI[pj]][:, 0:kn], func=AF.Relu),
                     reads=[b_P[PI[pj]]], writes=[b_rt[pj]])
                if h == 0:
                    S.op("dve", lambda e, pj=pj, k0=k0, kn=kn, i=i: e.tensor_scalar(
                        out=Isc[:, k0:k0 + kn], in0=rt[pj][:, 0:kn], scalar1=wq[:, i, 0:1], scalar2=None, op0=ALU.mult),
                        reads=[b_rt[pj], b_wq], writes=[b_Isc[kt]])
                else:
                    S.op("dve", lambda e, pj=pj, k0=k0, kn=kn, i=i, h=h: e.scalar_tensor_tensor(
                        out=Isc[:, k0:k0 + kn], in0=rt[pj][:, 0:kn], scalar=wq[:, i, h:h + 1], in1=Isc[:, k0:k0 + kn],
                        op0=ALU.mult, op1=ALU.add), reads=[b_rt[pj], b_wq, b_Isc[kt]], writes=[b_Isc[kt]])
        tiles = [b_Isc[kt] for kt in range(ntile)]
        S.op("dve", lambda e, nk=nk: e.tensor_reduce(out=sm[:, 0:1], in_=Isc[:, 0:nk], axis=AX.X, op=ALU.max, apply_absolute_value=True),
             reads=tiles, writes=[b_sm["M"]])
        S.op("pool", lambda e, nk=nk: e.tensor_tensor(out=Isc[:, nk - 256:nk], in0=Isc[:, nk - 256:nk], in1=cm[:], op=ALU.add),
             reads=[b_cm, b_Isc[ntile - 1]], writes=[b_Isc[ntile - 1]])
        S.op("dve", lambda e: e.tensor_scalar(out=sm[:, 1:2], in0=sm[:, 0:1], scalar1=2.0, scalar2=2.0, op0=ALU.mult, op1=ALU.add),
             reads=[b_sm["M"]], writes=[b_sm["W0"]])
        S.op("dve", lambda e: e.tensor_scalar(out=sm[:, 2:3], in0=sm[:, 0:1], scalar1=-1.0, scalar2=-1.0, op0=ALU.mult, op1=ALU.add),
             reads=[b_sm["M"]], writes=[b_sm["lo"]])
        S.op("dve", lambda e: e.scalar_tensor_tensor(out=sm[:, 3:4], in0=sm[:, 1:2], scalar=0.5, in1=sm[:, 2:3], op0=ALU.mult, op1=ALU.add),
             reads=[b_sm["W0"], b_sm["lo"]], writes=[b_sm["cand"]])
        for k in range(1, NITER + 1):
            S.op("dve", lambda e, nk=nk: e.tensor_scalar(out=mask[:, 0:nk], in0=Isc[:, 0:nk], scalar1=sm[:, 3:4], scalar2=None,
                                                         op0=ALU.is_ge, op1=ALU.add, accum_out=sm[:, 4:5]),
                 reads=tiles + [b_sm["cand"]], writes=[b_mask, b_sm["cnt"]])
            S.op("dve", lambda e: e.tensor_scalar(out=sm[:, 5:6], in0=sm[:, 4:5], scalar1=float(TOPK), scalar2=sm[:, 1:2],
                                                  op0=ALU.is_ge, op1=ALU.mult), reads=[b_sm["cnt"], b_sm["W0"]], writes=[b_sm["pw"]])
            S.op("dve", lambda e, k=k: e.scalar_tensor_tensor(out=sm[:, 2:3], in0=sm[:, 5:6], scalar=2.0 ** (-k), in1=sm[:, 2:3],
                                                              op0=ALU.mult, op1=ALU.add), reads=[b_sm["pw"], b_sm["lo"]], writes=[b_sm["lo"]])
            if k < NITER:
                S.op("dve", lambda e, k=k: e.scalar_tensor_tensor(out=sm[:, 3:4], in0=sm[:, 1:2], scalar=2.0 ** (-(k + 1)), in1=sm[:, 2:3],
                                                                  op0=ALU.mult, op1=ALU.add), reads=[b_sm["W0"], b_sm["lo"]], writes=[b_sm["cand"]])
        S.op("dve", lambda e, nk=nk: e.tensor_scalar(out=mask[:, 0:nk], in0=Isc[:, 0:nk], scalar1=sm[:, 2:3], scalar2=None, op0=ALU.is_ge),
             reads=tiles + [b_sm["lo"]], writes=[b_mask])

    def phaseT(i):
        nch = 2 * i + 2
        for g in range((nch + 7) // 8):
            n = min(8, nch - g * 8)
            mt = MT0 if (st["e"] % 2 == 0) else MT1; st["e"] += 1
            mtv = P[mt][:].bitcast(BF16)
            for cc in range(n):
                ch = g * 8 + cc
                S.op("pe", lambda e, mtv=mtv, cc=cc, ch=ch: e.transpose(mtv[:, cc * 128:(cc + 1) * 128], mask[:, ch * 128:(ch + 1) * 128], identb[:]),
                     reads=[b_mask, b_identb], writes=[b_P[mt]])
            S.op("act", lambda e, mtv=mtv, g=g, n=n: e.activation(out=maskT[:, g * 8:g * 8 + n, :].rearrange("p a b -> p (a b)"),
                                                                 in_=mtv[:, 0:n * 128], func=AF.Copy), reads=[b_P[mt]], writes=[b_maskT[g]])

    def phaseB(i):
        j = i % 2
        nch = 2 * i + 2
        for kc in range(nch):
            jj = kc % 2
            for eo in range(2):
                ps = PSe if eo == 0 else PSo
                po = POe if eo == 0 else POo
                S.op("pe", lambda e, eo=eo, kc=kc, ps=ps, j=j: e.matmul(
                    P[ps][:], lhsT=kT[eo * 64:(eo + 1) * 64, kc * 128:(kc + 1) * 128], rhs=qb_t[j][eo * 64:(eo + 1) * 64, :, :],
                    start=True, stop=True), reads=[b_kT, b_qb[j]], writes=[b_P[ps]])
                S.op("act", lambda e, jj=jj, eo=eo, ps=ps: e.activation(out=E[jj][eo][:], in_=P[ps][:], func=AF.Exp, scale=0.125),
                     reads=[b_P[ps]], writes=[b_E[jj][eo]])
                S.op("pool", lambda e, jj=jj, eo=eo, kc=kc: e.tensor_tensor(
                    out=PT[jj][eo][:].rearrange("p (a b) -> p a b", a=4), in0=E[jj][eo][:].rearrange("p (a b) -> p a b", a=4),
                    in1=maskT[:, kc:kc + 1, :].to_broadcast([128, 4, 128]), op=ALU.mult),
                    reads=[b_E[jj][eo], b_maskT[kc // 8]], writes=[b_PT[jj][eo]])
                S.op("pe", lambda e, jj=jj, eo=eo, kc=kc, po=po, nch=nch: e.matmul(
                    P[po][0:65, :], lhsT=v1[:, kc, :], rhs=PT[jj][eo][:], start=(kc == 0), stop=(kc == nch - 1)),
                    reads=[b_v1, b_PT[jj][eo]], writes=[b_P[po]])
        S.op("act", lambda e: e.activation(out=osb[:, 0:512], in_=P[POe][0:65, :], func=AF.Copy), reads=[b_P[POe]], writes=[b_osb])
        S.op("act", lambda e: e.activation(out=osb[:, 512:1024], in_=P[POo][0:65, :], func=AF.Copy), reads=[b_P[POo], b_osb], writes=[b_osb])
        S.op("dve", lambda e: e.reciprocal(out=rr[64:65, :], in_=osb[64:65, :]), reads=[b_osb], writes=[b_rr])
        for eo in range(2):
            mt = MT0 if (st["e"] % 2 == 0) else MT1; st["e"] += 1
            S.op("pe", lambda e, eo=eo, mt=mt: e.matmul(P[mt][0:64, :], lhsT=ones_row, rhs=rr[64:65, eo * 512:(eo + 1) * 512], start=True, stop=True),
                 reads=[b_cst, b_rr], writes=[b_P[mt]])
            S.op("dve", lambda e, eo=eo, mt=mt, j=j: e.tensor_tensor(out=ao[j][:, eo * 512:(eo + 1) * 512], in0=P[mt][0:64, :],
                                                                    in1=osb[0:64, eo * 512:(eo + 1) * 512], op=ALU.mult),
                 reads=[b_P[mt], b_osb, b_ao[j]], writes=[b_ao[j]])
        av = att_d.rearrange("d (c two) t -> d two c t", two=2)
        for eo in range(2):
            ob = S.buf(); outbufs.append(ob)
            S.dma("sp", av[:, eo, :, i * 128:(i + 1) * 128], ao[j][:, eo * 512:(eo + 1) * 512].rearrange("p (a b) -> p a b", a=4),
                  reads=[b_ao[j]], writes=[ob], owner=b_ao[j])

    load_q(0)
    phaseA(0)
    phaseT(0)
    for i in range(NQB):
        if i + 1 < NQB:
            load_q(i + 1)
            phaseA(i + 1)
        phaseB(i)
        if i + 1 < NQB:
            phaseT(i + 1)
    S.fence("sp", outbufs)
    S.emit()
    return nc, S


EPS = 1e-6


def build_ml(SEQ=8192):
    nc = bass.Bass("TRN2", target_bir_lowering=False)
    S = Sched(nc)
    NCHK = SEQ // 128
    HALF = SEQ // 2
    ISQ = 128.0 ** -0.5

    def din(name, shape, dt=F32):
        return nc.dram_tensor(name, list(shape), dt, kind="ExternalInput").ap()

    xqk_d = din("xqk", [4, 128, SEQ])
    cw_d = din("cw", [128, 4, 5])
    v_d = din("vtok", [SEQ, 256])
    o_d = din("otok", [SEQ, 256])
    g_d = din("gates", [SEQ, 4])
    gb_d = din("gb", [128, 4])
    mn_d = din("mn", [128, 256])
    mc_d = din("mconsts", [128, 384])
    hm_d = nc.dram_tensor("hm", [SEQ, 256], BF16, kind="ExternalOutput").ap()

    qks = [S.sb(f"qks{i}", [128, SEQ], BF16) for i in range(4)]; b_qks = [S.buf() for _ in range(4)]
    xin = S.sb("xin", [128, HALF + 3], F32); b_xin = S.buf()
    acc = S.sb("acc", [128, HALF], F32); b_acc = S.buf()
    cw = S.sb("cw", [128, 4, 5], F32); b_cw = S.buf()
    v1 = S.sb("v1", [128, NCHK, 2, 129], BF16); b_v1 = S.buf()
    gt = S.sb("gt", [128, NCHK, 4], F32); b_gt = S.buf()
    gb = S.sb("gb", [128, 4], F32); b_gb = S.buf()
    ngb = S.sb("ngb", [128, 4], F32); b_ngb = S.buf()
    mn = S.sb("mn", [128, 256], F32); b_mn = S.buf()
    mc = S.sb("mc", [128, 384], F32); b_mc = S.buf()
    identb = S.sb("identb", [128, 128], BF16); b_identb = S.buf()
    NN = NCHK * 2
    LF = S.sb("LF", [128, NN], F32); b_LF = S.buf()
    IT = S.sb("IT", [128, NN], F32); b_IT = S.buf()
    BC = S.sb("BC", [128, NN], F32); b_BC = S.buf()
    BLs = S.sb("BLs", [128, NN], F32); b_BL = S.buf()
    EB = S.sb("EB", [128, NN], F32); b_EB = S.buf()
    FB = S.sb("FB", [128, NN], F32); b_FB = S.buf()
    AE = S.sb("AE", [128, NN], F32); b_AE = S.buf()
    AC = S.sb("AC", [128, NN], F32); b_AC = S.buf()
    BV = S.sb("BV", [128, NN], F32); b_BV = S.buf()
    tmpn = S.sb("tmpn", [128, NN], F32); b_tmpn = S.buf()
    kw = [S.sb(f"kw{h}", [128, 128], BF16) for h in range(2)]; b_kw = [S.buf() for _ in range(2)]
    dg = [S.sb(f"dg{h}", [128, 128], F32) for h in range(2)]; b_dg = [S.buf() for _ in range(2)]
    DT = [S.sb(f"DT{h}", [128, 128], F32) for h in range(2)]; b_DT = [S.buf() for _ in range(2)]
    ST = [S.sb(f"ST{h}", [128, 128], BF16) for h in range(2)]; b_ST = [S.buf() for _ in range(2)]
    As = [S.sb(f"As{h}", [128, 129], F32) for h in range(2)]; b_As = [S.buf() for _ in range(2)]
    num = [S.sb(f"num{h}", [128, 129], F32) for h in range(2)]; b_num = [S.buf() for _ in range(2)]
    junk = [S.sb(f"junk{h}", [128, 128], F32) for h in range(2)]; b_junk = [S.buf() for _ in range(2)]
    sm = [S.sb(f"sm{h}", [128, 8], F32) for h in range(2)]
    b_s = [{k: S.buf() for k in ("dn", "rd", "ssq", "t", "ln", "rstd", "sc", "nd")} for _ in range(2)]
    Cf = [S.sb(f"Cf{h}", [128, 129], F32) for h in range(2)]; b_Cf = [S.buf() for _ in range(2)]
    Cb = [S.sb(f"Cb{h}", [128, 129], BF16) for h in range(2)]; b_Cb = [S.buf() for _ in range(2)]
    ot = [S.sb(f"ot{i}", [128, 256], F32) for i in range(2)]; b_ot = [S.buf() for _ in range(2)]
    go = [S.sb(f"go{i}", [128, 256], F32) for i in range(2)]; b_go = [S.buf() for _ in range(2)]
    ho = [S.sb(f"ho{i}", [128, 256], BF16) for i in range(2)]; b_ho = [S.buf() for _ in range(2)]
    P = [S.ps(f"P{i}", [128, 512], F32) for i in range(8)]; b_P = [S.buf() for _ in range(8)]
    TK, QK, BR, PA, PB, PU = 0, 1, 2, 3, 4, 5
    outbufs = []

    ident = mc[:, 0:128]
    tri = mc[:, 128:256]
    ones = mc[:, 256:384]

    S.dma("sp", mc[:], mc_d, writes=[b_mc])
    S.dma("sp", cw[:], cw_d, writes=[b_cw])
    S.dma("sp", gb[:], gb_d, writes=[b_gb])
    S.dma("sp", mn[:], mn_d, writes=[b_mn])
    S.dma("sp", gt[:], g_d.rearrange("(c p) g -> p c g", p=128), writes=[b_gt])
    S.op("dve", lambda e: e.memset(v1[:], 1.0), writes=[b_v1])
    vv = v_d.rearrange("(c p) (h e) -> p c h e", p=128, h=2)
    for g in range(0, NCHK, 8):
        for h in range(2):
            S.dma("pool", v1[:, g:g + 8, h, 0:128], vv[:, g:g + 8, h, :], writes=[b_v1])
    S.op("dve", lambda e: e.tensor_copy(out=identb[:], in_=ident), reads=[b_mc], writes=[b_identb])

    for slot in range(4):
        for hf in range(2):
            c0 = hf * HALF
            if hf == 0:
                S.op("pool", lambda e: e.memset(xin[:, 0:3], 0.0), writes=[b_xin])
                S.dma("sp", xin[:, 3:HALF + 3], xqk_d[slot][:, 0:HALF], writes=[b_xin])
            else:
                S.dma("sp", xin[:, 0:HALF + 3], xqk_d[slot][:, HALF - 3:SEQ], writes=[b_xin])
            S.op("dve", lambda e, slot=slot: e.tensor_scalar(out=acc[:], in0=xin[:, 0:HALF], scalar1=cw[:, slot, 0:1], scalar2=cw[:, slot, 4:5],
                                                             op0=ALU.mult, op1=ALU.add), reads=[b_xin, b_cw], writes=[b_acc])
            for j in range(1, 4):
                S.op("dve", lambda e, slot=slot, j=j: e.scalar_tensor_tensor(out=acc[:], in0=xin[:, j:j + HALF], scalar=cw[:, slot, j:j + 1], in1=acc[:],
                                                                           op0=ALU.mult, op1=ALU.add), reads=[b_xin, b_cw, b_acc], writes=[b_acc])
            S.op("act", lambda e, slot=slot, c0=c0: e.activation(out=qks[slot][:, c0:c0 + HALF], in_=acc[:], func=AF.Silu),
                 reads=[b_acc], writes=[b_qks[slot]])

    S.op("dve", lambda e: e.tensor_scalar(out=ngb[:], in0=gb[:], scalar1=-1.0, scalar2=None, op0=ALU.mult), reads=[b_gb], writes=[b_ngb])
    LFv = LF[:].rearrange("p (c h) -> p c h", h=2)
    ITv = IT[:].rearrange("p (c h) -> p c h", h=2)
    for h in range(2):
        S.op("dve", lambda e, h=h: e.tensor_scalar(out=ITv[:, :, h], in0=gt[:, :, h], scalar1=gb[:, h:h + 1], scalar2=None, op0=ALU.add),
             reads=[b_gt, b_gb, b_IT], writes=[b_IT])
        S.op("act", lambda e, h=h: e.activation(out=LFv[:, :, h], in_=gt[:, :, 2 + h], func=AF.Exp, bias=ngb[:, 2 + h:3 + h], scale=-1.0),
             reads=[b_gt, b_ngb, b_LF], writes=[b_LF])
    S.op("act", lambda e: e.activation(out=LF[:], in_=LF[:], func=AF.Ln, bias=1.0, scale=1.0), reads=[b_LF], writes=[b_LF])
    S.op("pe", lambda e: e.matmul(P[6][:, 0:NN], lhsT=tri, rhs=LF[:], start=True, stop=True), reads=[b_mc, b_LF], writes=[b_P[6]])
    S.op("pe", lambda e: e.matmul(P[7][:, 0:NN], lhsT=ones, rhs=LF[:], start=True, stop=True), reads=[b_mc, b_LF], writes=[b_P[7]])
    S.op("act", lambda e: e.activation(out=BC[:], in_=P[6][:, 0:NN], func=AF.Copy), reads=[b_P[6]], writes=[b_BC])
    S.op("act", lambda e: e.activation(out=BLs[:], in_=P[7][:, 0:NN], func=AF.Copy), reads=[b_P[7]], writes=[b_BL])
    S.op("act", lambda e: e.activation(out=EB[:], in_=BC[:], func=AF.Exp, scale=-1.0), reads=[b_BC], writes=[b_EB])
    S.op("act", lambda e: e.activation(out=FB[:], in_=BLs[:], func=AF.Exp, scale=-1.0), reads=[b_BL], writes=[b_FB])
    S.op("dve", lambda e: e.tensor_tensor(out=AC[:], in0=IT[:], in1=BC[:], op=ALU.add), reads=[b_IT, b_BC], writes=[b_AC])
    S.op("dve", lambda e: e.tensor_tensor(out=tmpn[:], in0=AC[:], in1=BLs[:], op=ALU.subtract), reads=[b_AC, b_BL], writes=[b_tmpn])
    S.op("act", lambda e: e.activation(out=AE[:], in_=tmpn[:], func=AF.Exp), reads=[b_tmpn], writes=[b_AE])
    S.op("dve", lambda e: e.tensor_scalar(out=BV[:], in0=BC[:], scalar1=-1.0, scalar2=None, op0=ALU.mult), reads=[b_BC], writes=[b_BV])

    for h in range(2):
        S.op("dve", lambda e, h=h: e.memset(Cf[h][:], 0.0), writes=[b_Cf[h]])
        S.op("dve", lambda e, h=h: e.memset(Cb[h][:], 0.0), writes=[b_Cb[h]])

    for c in range(NCHK):
        cs = slice(c * 128, (c + 1) * 128)
        oj = c % 2
        S.dma("sp", ot[oj][:], o_d[cs, :], writes=[b_ot[oj]])
        S.op("act", lambda e, oj=oj: e.activation(out=go[oj][:], in_=ot[oj][:], func=AF.Exp, scale=-1.0), reads=[b_ot[oj]], writes=[b_go[oj]])
        S.op("dve", lambda e, oj=oj: e.tensor_scalar(out=go[oj][:], in0=go[oj][:], scalar1=1.0, scalar2=None, op0=ALU.add), reads=[b_go[oj]], writes=[b_go[oj]])
        S.op("dve", lambda e, oj=oj: e.reciprocal(out=go[oj][:], in_=go[oj][:]), reads=[b_go[oj]], writes=[b_go[oj]])
        S.op("dve", lambda e, oj=oj: e.tensor_tensor(out=go[oj][:], in0=go[oj][:], in1=mn[:], op=ALU.mult), reads=[b_go[oj], b_mn], writes=[b_go[oj]])
        for h in range(2):
            n = 2 * c + h
            q_ = qks[h]; k_ = qks[2 + h]
            bq = b_qks[h]; bk = b_qks[2 + h]
            tkv = P[TK][:].bitcast(BF16)
            S.op("pe", lambda e, k_=k_, cs=cs, tkv=tkv: e.transpose(tkv[:, 0:128], k_[:, cs], identb[:]), reads=[bk, b_identb], writes=[b_P[TK]])
            S.op("dve", lambda e, h=h, n=n, tkv=tkv: e.tensor_scalar(out=kw[h][:], in0=tkv[:, 0:128], scalar1=AE[:, n:n + 1], scalar2=ISQ,
                                                                      op0=ALU.mult, op1=ALU.mult), reads=[b_P[TK], b_AE], writes=[b_kw[h]])
            S.op("pe", lambda e, k_=k_, q_=q_, cs=cs: e.matmul(P[QK][:, 0:128], lhsT=k_[:, cs], rhs=q_[:, cs], start=True, stop=True),
                 reads=[bk, bq], writes=[b_P[QK]])
            S.op("dve", lambda e, h=h, n=n: e.tensor_scalar(out=dg[h][:], in0=ident, scalar1=BV[:, n:n + 1], scalar2=None, op0=ALU.mult),
                 reads=[b_mc, b_BV], writes=[b_dg[h]])
            S.op("pe", lambda e, h=h: e.matmul(P[BR][:, 0:128], lhsT=ones, rhs=dg[h][:], start=True, stop=True), reads=[b_mc, b_dg[h]], writes=[b_P[BR]])
            S.op("act", lambda e, h=h, n=n: e.activation(out=DT[h][:], in_=P[BR][:, 0:128], func=AF.Exp, bias=AC[:, n:n + 1], scale=1.0),
                 reads=[b_P[BR], b_AC], writes=[b_DT[h]])
            S.op("pool", lambda e, h=h: e.affine_select(out=DT[h][:], in_=DT[h][:], pattern=[[1, 128]], compare_op=ALU.is_ge, fill=0.0,
                                                        base=0, channel_multiplier=-1), reads=[b_DT[h]], writes=[b_DT[h]])
            S.op("dve", lambda e, h=h: e.scalar_tensor_tensor(out=ST[h][:], in0=P[QK][:, 0:128], scalar=ISQ, in1=DT[h][:], op0=ALU.mult, op1=ALU.mult),
                 reads=[b_P[QK], b_DT[h]], writes=[b_ST[h]])
            S.op("pe", lambda e, h=h, c=c: e.matmul(P[PA][:, 0:129], lhsT=ST[h][:], rhs=v1[:, c, h, :], start=True, stop=True),
                 reads=[b_ST[h], b_v1], writes=[b_P[PA]])
            S.op("pe", lambda e, h=h, q_=q_, cs=cs: e.matmul(P[PB][:, 0:129], lhsT=q_[:, cs], rhs=Cb[h][:], start=True, stop=True),
                 reads=[bq, b_Cb[h]], writes=[b_P[PB]])
            S.op("act", lambda e, h=h: e.activation(out=As[h][:], in_=P[PA][:, 0:129], func=AF.Copy), reads=[b_P[PA]], writes=[b_As[h]])
            S.op("dve", lambda e, h=h, n=n: e.scalar_tensor_tensor(out=num[h][:], in0=P[PB][:, 0:129], scalar=EB[:, n:n + 1], in1=As[h][:],
                                                                   op0=ALU.mult, op1=ALU.add), reads=[b_P[PB], b_EB, b_As[h]], writes=[b_num[h]])
            s_ = sm[h]; bs = b_s[h]
            S.op("dve", lambda e, h=h, s_=s_: e.tensor_scalar(out=s_[:, 7:8], in0=num[h][:, 128:129], scalar1=-1.0, scalar2=1.0, op0=ALU.mult, op1=ALU.max),
                 reads=[b_num[h]], writes=[bs["nd"]])
            S.op("dve", lambda e, h=h, s_=s_: e.tensor_tensor(out=s_[:, 0:1], in0=num[h][:, 128:129], in1=s_[:, 7:8], op=ALU.max),
                 reads=[b_num[h], bs["nd"]], writes=[bs["dn"]])
            S.op("dve", lambda e, s_=s_: e.reciprocal(out=s_[:, 1:2], in_=s_[:, 0:1]), reads=[bs["dn"]], writes=[bs["rd"]])
            S.op("dve", lambda e, h=h, s_=s_: e.scalar_tensor_tensor(out=junk[h][:], in0=num[h][:, 0:128], scalar=1.0, in1=num[h][:, 0:128],
                                                                     op0=ALU.mult, op1=ALU.mult, accum_out=s_[:, 2:3]),
                 reads=[b_num[h]], writes=[b_junk[h], bs["ssq"]])
            S.op("dve", lambda e, s_=s_: e.scalar_tensor_tensor(out=s_[:, 3:4], in0=s_[:, 1:2], scalar=s_[:, 1:2], in1=s_[:, 2:3], op0=ALU.mult, op1=ALU.mult),
                 reads=[bs["rd"], bs["ssq"]], writes=[bs["t"]])
            S.op("act", lambda e, s_=s_: e.activation(out=s_[:, 4:5], in_=s_[:, 3:4], func=AF.Ln, bias=EPS, scale=1.0 / 128), reads=[bs["t"]], writes=[bs["ln"]])
            S.op("act", lambda e, s_=s_: e.activation(out=s_[:, 5:6], in_=s_[:, 4:5], func=AF.Exp, scale=-0.5), reads=[bs["ln"]], writes=[bs["rstd"]])
            S.op("dve", lambda e, s_=s_: e.tensor_tensor(out=s_[:, 6:7], in0=s_[:, 5:6], in1=s_[:, 1:2], op=ALU.mult), reads=[bs["rstd"], bs["rd"]], writes=[bs["sc"]])
            S.op("dve", lambda e, h=h, s_=s_, oj=oj: e.scalar_tensor_tensor(out=ho[oj][:, h * 128:(h + 1) * 128], in0=num[h][:, 0:128], scalar=s_[:, 6:7],
                                                                            in1=go[oj][:, h * 128:(h + 1) * 128], op0=ALU.mult, op1=ALU.mult),
                 reads=[b_num[h], bs["sc"], b_go[oj], b_ho[oj]], writes=[b_ho[oj]])
            S.op("pe", lambda e, h=h, c=c: e.matmul(P[PU][:, 0:129], lhsT=kw[h][:], rhs=v1[:, c, h, :], start=True, stop=True),
                 reads=[b_kw[h], b_v1], writes=[b_P[PU]])
            S.op("dve", lambda e, h=h, n=n: e.scalar_tensor_tensor(out=Cf[h][:], in0=Cf[h][:], scalar=FB[:, n:n + 1], in1=P[PU][:, 0:129],
                                                                   op0=ALU.mult, op1=ALU.add), reads=[b_Cf[h], b_FB, b_P[PU]], writes=[b_Cf[h]])
            S.op("act", lambda e, h=h: e.activation(out=Cb[h][:], in_=Cf[h][:], func=AF.Copy), reads=[b_Cf[h]], writes=[b_Cb[h]])
        ob = S.buf(); outbufs.append(ob)
        S.dma("sp", hm_d[cs, :], ho[oj][:], reads=[b_ho[oj]], writes=[ob], owner=b_ho[oj])
    S.fence("sp", outbufs)
    S.emit()
    return nc, S


import ml_dtypes as _mld

_BF = _mld.bfloat16
_SEQ = 8192
_NB = 4
_T = 4096
_PROGS = {}


def _prog(key, fn):
    if key not in _PROGS:
        _PROGS[key] = fn()[0]
    return _PROGS[key]


def _att_consts():
    c = np.zeros((128, 256), np.float32)
    c[:, 0:128] = np.eye(128)
    c[:, 128:256] = 1.0
    return c


def _cmask(r):
    tri = np.where(np.arange(128)[None, :] <= np.arange(128)[:, None], 0.0, NEG).astype(np.float32)
    if r == 1:
        return np.concatenate([np.zeros((128, 128), np.float32), tri], 1)
    return np.concatenate([tri, np.full((128, 128), NEG, np.float32)], 1)


def _tokmap(r, TQ):
    tl = np.arange(TQ)
    return (2 * (tl // 128) + r) * 128 + tl % 128


def _ml_consts():
    c = np.zeros((128, 384), np.float32)
    c[:, 0:128] = np.eye(128)
    c[:, 128:256] = (np.arange(128)[:, None] <= np.arange(128)[None, :]).astype(np.float32)
    c[:, 256:384] = 1.0
    return c


def _pre_weights(inp, l):
    return dict(wgu1=prep_wgu(inp["ffn1_w_gate"][l], inp["ffn1_w_up"][l]), wd1=prep_wd(inp["ffn1_w_down"][l]),
                gn1=prep_g(inp["ffn1_norm"][l]), gnm=prep_g(inp["mix_norm"][l]),
                winF=prep_winF(inp["w_in"][l]), winT=prep_winT(inp["w_in"][l]), gqk=prep_gqk(inp["q_norm"][l], inp["k_norm"][l]))


def _post_weights(inp, l):
    return dict(wout=prep_wout(inp["w_out"][l]), wgu2=prep_wgu(inp["ffn2_w_gate"][l], inp["ffn2_w_up"][l]),
                wd2=prep_wd(inp["ffn2_w_down"][l]), gn2=prep_g(inp["ffn2_norm"][l]))


def _run(nc, maps):
    res = run_bass_kernel_spmd(nc, maps, core_ids=list(range(8)))
    return res.results


def _mixer(inp, l, tok_res):
    TQ = _SEQ // 2
    NQB = TQ // 128
    att_maps, ml_maps = [], []
    seqs = []
    for b in range(_NB):
        r0, r1 = tok_res[2 * b], tok_res[2 * b + 1]
        fbf = np.concatenate([r0["fbf"], r1["fbf"]], 2)
        qkm = np.concatenate([r0["qkm"], r1["qkm"]], 2)
        tokT = np.concatenate([r0["tokT"], r1["tokT"]], 0)
        seqs.append((fbf, qkm, tokT))
    ac = _att_consts()
    mc = _ml_consts()
    for b in range(_NB):
        fbf, qkm, tokT = seqs[b]
        for r in range(2):
            tm = _tokmap(r, TQ)
            att_maps.append(dict(
                kT=np.ascontiguousarray(fbf[6]), kiT=np.ascontiguousarray(fbf[7]), vtok=np.ascontiguousarray(tokT[:, 0:64]),
                qT=np.ascontiguousarray(fbf[0:4][:, :, tm].transpose(1, 0, 2)), qiT=np.ascontiguousarray(fbf[4:6][:, :, tm].transpose(1, 0, 2)),
                widx=np.ascontiguousarray(tokT[tm, 64:68].reshape(NQB, 128, 4).transpose(1, 0, 2)), cmask=_cmask(r), aconsts=ac))
        for hp in range(2):
            chs = [2 * hp, 2 * hp + 1, 4 + 2 * hp, 5 + 2 * hp]
            cw = np.zeros((128, 4, 5), np.float32)
            for s, ch in enumerate(chs):
                cw[:, s, 0:4] = inp["conv_w"][l][:, 0, ch * 128:(ch + 1) * 128].T
                cw[:, s, 4] = inp["conv_b"][l][ch * 128:(ch + 1) * 128]
            gcols = [68 + 2 * hp, 69 + 2 * hp, 72 + 2 * hp, 73 + 2 * hp]
            gbv = inp["gate_b"][l][[2 * hp, 2 * hp + 1, 4 + 2 * hp, 5 + 2 * hp]]
            ml_maps.append(dict(
                xqk=np.ascontiguousarray(qkm[chs]), cw=cw, vtok=np.ascontiguousarray(tokT[:, 76 + 256 * hp:76 + 256 * (hp + 1)]),
                otok=np.ascontiguousarray(tokT[:, 588 + 256 * hp:588 + 256 * (hp + 1)]), gates=np.ascontiguousarray(tokT[:, gcols]),
                gb=np.ascontiguousarray(np.tile(gbv[None, :], (128, 1))).astype(np.float32),
                mn=np.ascontiguousarray(np.tile(inp["m_norm"][l][2 * hp:2 * hp + 2].reshape(1, 256), (128, 1))).astype(np.float32), mconsts=mc))
    att_res = _run(_prog("att", lambda: build_att(_SEQ)), att_maps)
    ml_res = _run(_prog("ml", lambda: build_ml(_SEQ)), ml_maps)
    mix = []
    for b in range(_NB):
        m = np.zeros((1024, _SEQ), _BF)
        for r in range(2):
            tm = _tokmap(r, TQ)
            a = att_res[2 * b + r]["attT"]
            m[0:512][:, tm] = a.transpose(1, 0, 2).reshape(512, TQ)
        for hp in range(2):
            m[512 + 256 * hp:512 + 256 * (hp + 1), :] = ml_res[2 * b + hp]["hm"].T
        for r in range(2):
            mix.append(np.ascontiguousarray(m[:, r * _T:(r + 1) * _T]))
    return mix


def kernel(**inp):
    inp = {k: np.asarray(v) for k, v in inp.items()}
    x = inp["x"]
    consts = make_consts()
    rope = []
    xT = []
    for c in range(8):
        b, r = c // 2, c % 2
        pos = np.arange(r * _T, (r + 1) * _T)
        rope.append(rope_tables(pos))
        xT.append(np.ascontiguousarray(x[b, r * _T:(r + 1) * _T, :].T))
    w0 = _pre_weights(inp, 0)
    maps = [dict(xin=xT[c], consts=consts, cosT=rope[c][0], sinT=rope[c][1], **w0) for c in range(8)]
    res = _run(_prog("pre", lambda: build_tok(False, True, _T)), maps)
    x1 = [res[c]["x1o"] for c in range(8)]
    mix = _mixer(inp, 0, res)
    p0 = _post_weights(inp, 0)
    w1 = _pre_weights(inp, 1)
    maps = [dict(xin=x1[c], mix=mix[c], consts=consts, cosT=rope[c][0], sinT=rope[c][1], **p0, **w1) for c in range(8)]
    res = _run(_prog("postpre", lambda: build_tok(True, True, _T)), maps)
    x1 = [res[c]["x1o"] for c in range(8)]
    mix = _mixer(inp, 1, res)
    p1 = _post_weights(inp, 1)
    maps = [dict(xin=x1[c], mix=mix[c], consts=consts, **p1) for c in range(8)]
    res = _run(_prog("post", lambda: build_tok(True, False, _T)), maps)
    out = np.zeros((_NB, _SEQ, 1024), np.float32)
    for c in range(8):
        b, r = c // 2, c % 2
        out[b, r * _T:(r + 1) * _T, :] = res[c]["xo"].T
    return out
```
